# Optimizing a Trainium2 kernel written in Bass

```python
import math
import jax, jax.numpy as jnp
from jax import lax
import numpy as np

D_MODEL = 1024
BATCH = 4
SEQ = 8192
DEPTH = 1

MLA_HEADS = 8
MLA_Q_RANK = 384
MLA_KV_RANK = 256
MLA_NOPE_DIM = 128
MLA_ROPE_DIM = 64
MLA_V_DIM = D_MODEL // MLA_HEADS
ROPE_THETA = 10000.0
Q_BLOCK = 128
DIL_GROUPS = ((128, 1), (512, 4), (2048, 16))
DIL_HEADS = 8
DIL_HEAD_DIM = D_MODEL // DIL_HEADS
N_DIL_HEADS = len(DIL_GROUPS) * DIL_HEADS
REL_BUCKETS = 32
REL_MAX_DIST = 1024
N_EXPERTS = 16
EC_CAPACITY_FACTOR = 2
D_FF_EXPERT = 2 * D_MODEL
N_BRANCHES = 2
NORM_EPS = 1e-6
NEG_INF = -1e30
COLS_DIL = len(DIL_GROUPS) * 3 * DIL_HEADS * DIL_HEAD_DIM
SPLIT_SIZES = (MLA_Q_RANK, MLA_KV_RANK, MLA_ROPE_DIM, COLS_DIL, N_BRANCHES * D_MODEL)
D_IN = sum(SPLIT_SIZES)

kernel_name = 'hybrid_mla_dilated_ec_moe_block'


def rmsnorm(t, g):
    tf = t.astype(jnp.float32)
    y = tf * lax.rsqrt(jnp.mean(tf * tf, axis=-1, keepdims=True) + NORM_EPS)
    return (y * g.astype(jnp.float32)).astype(t.dtype)


def rope(t, cos, sin):
    t1, t2 = jnp.split(t, 2, axis=-1)
    return jnp.concatenate([t1 * cos - t2 * sin, t1 * sin + t2 * cos], axis=-1).astype(t.dtype)


def t5_bucket(rel):
    nb = REL_BUCKETS // 2
    ret = jnp.where(rel > 0, nb, 0)
    n = jnp.abs(rel)
    max_exact = nb // 2
    nf = jnp.maximum(n, 1).astype(jnp.float32)
    large = max_exact + (jnp.log(nf / max_exact) / math.log(REL_MAX_DIST / max_exact)
                         * (nb - max_exact)).astype(jnp.int32)
    large = jnp.minimum(large, nb - 1)
    return ret + jnp.where(n < max_exact, n, large)


def mla_attention(c_q, c_kv, k_pe, g_q_lat, g_kv_lat, w_uq, w_ukv, cos, sin):
    B, S, _ = c_q.shape
    q = (rmsnorm(c_q, g_q_lat) @ w_uq).reshape(B, S, MLA_HEADS, MLA_NOPE_DIM + MLA_ROPE_DIM)
    q_nope = q[..., :MLA_NOPE_DIM]
    q_pe = rope(q[..., MLA_NOPE_DIM:], cos[:, :, None], sin[:, :, None])
    kv = (rmsnorm(c_kv, g_kv_lat) @ w_ukv).reshape(B, S, MLA_HEADS, MLA_NOPE_DIM + MLA_V_DIM)
    k_nope, v = kv[..., :MLA_NOPE_DIM], kv[..., MLA_NOPE_DIM:]
    k_pe = rope(k_pe, cos, sin)
    scale = (MLA_NOPE_DIM + MLA_ROPE_DIM) ** -0.5
    nqb = S // Q_BLOCK

    def to_blocks(t):
        return jnp.moveaxis(t.reshape((B, nqb, Q_BLOCK) + t.shape[2:]), 1, 0)

    def attend(blk):
        qn, qp = blk
        s = (jnp.einsum('bqhc,bkhc->bhqk', qn, k_nope)
             + jnp.einsum('bqhr,bkr->bhqk', qp, k_pe))
        p = jax.nn.softmax(s.astype(jnp.float32) * scale, axis=-1).astype(v.dtype)
        return jnp.einsum('bhqk,bkhc->bqhc', p, v)

    o = lax.map(attend, (to_blocks(q_nope), to_blocks(q_pe)))
    return jnp.moveaxis(o, 0, 1).reshape(B, S, MLA_HEADS * MLA_V_DIM)


def dilated_band_attention(q, k, v, pos, bias_table, window, dilation):
    B, S, H, Dh = q.shape
    d = dilation
    r = window // (2 * d)
    L = S // d
    nb = -(-L // r)
    Lp = nb * r

    def to_classes(t):
        t = jnp.moveaxis(t.reshape((B, L, d) + t.shape[2:]), 2, 1)
        return jnp.pad(t, [(0, 0), (0, 0), (0, Lp - L)] + [(0, 0)] * (t.ndim - 3))

    def band(t):
        t = jnp.pad(t, [(0, 0), (0, 0), (r, r)] + [(0, 0)] * (t.ndim - 3))
        t = t.reshape((B, d, nb + 2, r) + t.shape[3:])
        return jnp.concatenate([t[:, :, :-2], t[:, :, 1:-1], t[:, :, 2:]], axis=3)

    qc, kc, vc, pc = to_classes(q), to_classes(k), to_classes(v), to_classes(pos)
    kb, vb, pkb = band(kc), band(vc), band(pc)
    qb = qc.reshape(B, d, nb, r, H, Dh)
    pqb = pc.reshape(B, d, nb, r)
    s = jnp.einsum('bgnqhc,bgnkhc->bgnhqk', qb, kb).astype(jnp.float32) * (Dh ** -0.5)
    rel = pkb[:, :, :, None, :] - pqb[..., None]
    bias = bias_table[t5_bucket(rel)]
    s = s + jnp.moveaxis(bias, -1, 3).astype(jnp.float32)
    qi = jnp.arange(nb)[:, None, None] * r + jnp.arange(r)[None, :, None]
    kj = (jnp.arange(nb)[:, None, None] - 1) * r + jnp.arange(3 * r)[None, None, :]
    valid = (jnp.abs(kj - qi) <= r) & (kj >= 0) & (kj < L)
    s = jnp.where(valid[:, None], s, NEG_INF)
    m = jnp.max(s, axis=-1, keepdims=True)
    p = jnp.exp(s - m)
    den = jnp.sum(p, axis=-1, keepdims=True)
    o = jnp.einsum('bgnhqk,bgnkhc->bgnqhc', (p / den).astype(v.dtype), vb)
    lse = (m + jnp.log(den))[..., 0]
    o = o.reshape(B, d, Lp, H, Dh)[:, :, :L]
    o = jnp.moveaxis(o, 1, 2).reshape(B, S, H, Dh)
    lse = jnp.moveaxis(lse, 3, 4).reshape(B, d, Lp, H)[:, :, :L]
    lse = jnp.moveaxis(lse, 1, 2).reshape(B, S, H)
    return o, lse


def expert_choice_ffn(h, w_router, w_gate, w_up, w_down):
    B, S, D = h.shape
    cap = EC_CAPACITY_FACTOR * S // N_EXPERTS
    aff = jax.nn.softmax((h @ w_router).astype(jnp.float32), axis=-1)
    gate, idx = lax.top_k(jnp.swapaxes(aff, 1, 2), cap)
    bidx = jnp.arange(B)[:, None, None]
    xe = h[bidx, idx]
    hid = (jax.nn.silu(jnp.einsum('becd,edf->becf', xe, w_gate))
           * jnp.einsum('becd,edf->becf', xe, w_up))
    ye = jnp.einsum('becf,efd->becd', hid, w_down) * gate[..., None].astype(h.dtype)
    return jnp.zeros_like(h).at[bidx, idx].add(ye)


def setup_inputs(seed: int = 0) -> dict:
    key = jax.random.key(seed)
    ks = jax.random.split(key, 20)
    f32 = jnp.float32

    def nrm(k, shape, s):
        return jax.random.normal(k, shape, f32) * s

    def gain(k, shape):
        return 1.0 + 0.02 * jax.random.normal(k, shape, f32)

    x = nrm(ks[0], (BATCH, SEQ, D_MODEL), 1.0)
    c = nrm(ks[1], (BATCH, D_MODEL), 1.0)
    offs = jax.random.randint(ks[2], (BATCH, 1), 0, 4096, dtype=jnp.int32)
    positions = offs + jnp.arange(SEQ, dtype=jnp.int32)[None, :]
    w_ada = nrm(ks[3], (DEPTH, D_MODEL, 6 * D_MODEL), 0.02)
    b_ada = nrm(ks[4], (DEPTH, 6 * D_MODEL), 0.02)
    g_norm_mix = gain(ks[5], (DEPTH, D_MODEL))
    w_in = nrm(ks[6], (DEPTH, D_MODEL, D_IN), D_MODEL ** -0.5)
    g_q_lat = gain(ks[7], (DEPTH, MLA_Q_RANK))
    g_kv_lat = gain(ks[8], (DEPTH, MLA_KV_RANK))
    w_uq = nrm(ks[9], (DEPTH, MLA_Q_RANK, MLA_HEADS * (MLA_NOPE_DIM + MLA_ROPE_DIM)), MLA_Q_RANK ** -0.5)
    w_ukv = nrm(ks[10], (DEPTH, MLA_KV_RANK, MLA_HEADS * (MLA_NOPE_DIM + MLA_V_DIM)), MLA_KV_RANK ** -0.5)
    rel_bias = nrm(ks[11], (REL_BUCKETS, N_DIL_HEADS), 0.5)
    w_out = nrm(ks[12], (DEPTH, D_MODEL, D_MODEL), D_MODEL ** -0.5)
    g_norm_ffn = gain(ks[13], (DEPTH, D_MODEL))
    w_router = nrm(ks[14], (DEPTH, D_MODEL, N_EXPERTS), D_MODEL ** -0.5)
    w_gate = nrm(ks[15], (DEPTH, N_EXPERTS, D_MODEL, D_FF_EXPERT), D_MODEL ** -0.5)
    w_up = nrm(ks[16], (DEPTH, N_EXPERTS, D_MODEL, D_FF_EXPERT), D_MODEL ** -0.5)
    w_down = nrm(ks[17], (DEPTH, N_EXPERTS, D_FF_EXPERT, D_MODEL), D_FF_EXPERT ** -0.5)
    g_final = gain(ks[18], (D_MODEL,))
    return {'x': x, 'c': c, 'positions': positions, 'w_ada': w_ada, 'b_ada': b_ada,
            'g_norm_mix': g_norm_mix, 'w_in': w_in, 'g_q_lat': g_q_lat, 'g_kv_lat': g_kv_lat,
            'w_uq': w_uq, 'w_ukv': w_ukv, 'rel_bias': rel_bias, 'w_out': w_out,
            'g_norm_ffn': g_norm_ffn, 'w_router': w_router, 'w_gate': w_gate, 'w_up': w_up,
            'w_down': w_down, 'g_final': g_final}


def reference(x, c, positions, w_ada, b_ada, g_norm_mix, w_in, g_q_lat, g_kv_lat, w_uq, w_ukv,
              rel_bias, w_out, g_norm_ffn, w_router, w_gate, w_up, w_down, g_final):
    B, S, D = x.shape
    inv_freq = ROPE_THETA ** (-jnp.arange(0, MLA_ROPE_DIM, 2, dtype=jnp.float32) / MLA_ROPE_DIM)
    ang = positions.astype(jnp.float32)[..., None] * inv_freq
    cos, sin = jnp.cos(ang), jnp.sin(ang)
    cond = jax.nn.silu(c)
    split_at = [int(v) for v in np.cumsum(SPLIT_SIZES)[:-1]]
    for l in range(DEPTH):
        mod = (cond @ w_ada[l] + b_ada[l])[:, None, :]
        sh1, sc1, gt1, sh2, sc2, gt2 = jnp.split(mod, 6, axis=-1)
        h = rmsnorm(x, g_norm_mix[l]) * (1.0 + sc1) + sh1
        c_q, c_kv, k_pe, dil, gates = jnp.split(h @ w_in[l], split_at, axis=-1)
        o_a = mla_attention(c_q, c_kv, k_pe, g_q_lat[l], g_kv_lat[l], w_uq[l], w_ukv[l], cos, sin)
        dil = dil.reshape(B, S, len(DIL_GROUPS), 3, DIL_HEADS, DIL_HEAD_DIM)
        outs, lses = [], []
        for gi, (win, dd) in enumerate(DIL_GROUPS):
            o_g, lse_g = dilated_band_attention(
                dil[:, :, gi, 0], dil[:, :, gi, 1], dil[:, :, gi, 2], positions,
                rel_bias[:, gi * DIL_HEADS:(gi + 1) * DIL_HEADS], win, dd)
            outs.append(o_g)
            lses.append(lse_g)
        wts = jax.nn.softmax(jnp.stack(lses), axis=0)
        o_b = jnp.einsum('gbsh,gbshc->bshc', wts.astype(x.dtype), jnp.stack(outs)).reshape(B, S, D)
        g_a, g_b = jnp.split(jax.nn.sigmoid(gates), 2, axis=-1)
        x = x + gt1 * ((g_a * o_a + g_b * o_b) @ w_out[l])
        h2 = rmsnorm(x, g_norm_ffn[l]) * (1.0 + sc2) + sh2
        x = x + gt2 * expert_choice_ffn(h2, w_router[l], w_gate[l], w_up[l], w_down[l])
    return rmsnorm(x, g_final)
```

```python
import math
from contextlib import ExitStack

import numpy as np
import ml_dtypes
import concourse.bass as bass
import concourse.mybir as mybir
from concourse.bass_utils import run_bass_kernel_spmd

F32 = mybir.dt.float32
BF16 = mybir.dt.bfloat16
I32 = mybir.dt.int32
AF = mybir.ActivationFunctionType
ALU = mybir.AluOpType
AX = mybir.AxisListType

NCORES = 8
D = 1024
S = 8192
NO = 4096
EPS = 1e-6
NEG = -30000.0
TWO_PI_HI = 6.28125
TWO_PI_LO = 6.283185307179586 - 6.28125
DIL = ((128, 1), (512, 4), (2048, 16))
def SS(a, cnt, d):
    return slice(a, a + (cnt - 1) * d + 1, d)


DBG_SEL = (0, 0, 0, 1)
HPAD = 1024


class _Tok:
    __slots__ = ("sem", "val", "eng")

    def __init__(self, sem, val, eng):
        self.sem, self.val, self.eng = sem, val, eng


class _Rec:
    __slots__ = ("waits", "fn", "inc", "cntval", "is_dma")

    def __init__(self, waits, fn, inc=None, is_dma=False):
        self.waits, self.fn, self.inc, self.cntval, self.is_dma = waits, fn, inc, None, is_dma


class KB:
    RING = 8
    CE = ("pe", "act", "dve", "pool")

    def __init__(self, nc):
        self.nc = nc
        self.psem = {e: nc.alloc_semaphore("pg_" + e) for e in self.CE}
        self.pcnt = {e: 0 for e in self.CE}
        self.dsem = {q: [nc.alloc_semaphore(f"dq_{q}_{i}") for i in range(self.RING)]
                     for q in ("sp", "act", "pool")}
        self.dcnt = {q: [0] * self.RING for q in self.dsem}
        self.dnext = {q: 0 for q in self.dsem}
        self.seen = {e: {} for e in ("pe", "act", "dve", "pool", "sp")}
        self.csem = nc.alloc_semaphore("coll_sem")
        self.ccnt = 0
        self._reset()

    def _reset(self):
        self.ops = {e: [] for e in ("pe", "act", "dve", "pool", "sp")}
        self.pending = {e: [] for e in self.CE}
        self.lastw = {}
        self.readers = {}

    def _resolve(self, tok):
        if tok.val is None:
            eng = tok.eng
            rec = self.ops[eng][-1]
            assert not rec.is_dma and rec.fn is not None
            if rec.inc is None:
                self.pcnt[eng] += 1
                rec.inc = (self.psem[eng], 1)
                rec.cntval = self.pcnt[eng]
            for t in self.pending[eng]:
                t.val = rec.cntval
            self.pending[eng] = []
        return tok.val

    def _deps(self, eng, reads, writes):
        toks = []
        for k in reads:
            t = self.lastw.get(k)
            if t is not None:
                toks.append(t)
        for k in writes:
            t = self.lastw.get(k)
            if t is not None:
                toks.append(t)
            toks.extend(self.readers.get(k, ()))
        need = {}
        for t in toks:
            if t.eng == eng and eng == "pe":
                continue
            v = self._resolve(t)
            if need.get(t.sem, (0, None))[0] < v:
                need[t.sem] = (v, t.sem)
        out = []
        seen = self.seen[eng]
        for key, (v, sem) in need.items():
            if seen.get(key, 0) < v:
                seen[key] = v
                out.append((sem, v))
        return out

    def _record(self, tok, reads, writes):
        for k in reads:
            self.readers.setdefault(k, []).append(tok)
        for k in writes:
            self.lastw[k] = tok
            self.readers[k] = []

    def op(self, eng, fn, r=(), w=()):
        waits = self._deps(eng, r, w)
        tok = _Tok(self.psem[eng], None, eng)
        self.ops[eng].append(_Rec(waits, fn))
        self.pending[eng].append(tok)
        self._record(tok, r, w)
        return tok

    def dma(self, q, out, in_, r=(), w=(), **kw):
        if q in self.pending and self.pending[q]:
            self._resolve(self.pending[q][0])
        i = self.dnext[q]
        self.dnext[q] = (i + 1) % self.RING
        sem = self.dsem[q][i]
        waits = self._deps(q, r, w)
        prev = self.dcnt[q][i]
        if prev > 0 and self.seen[q].get(sem, 0) < prev:
            self.seen[q][sem] = prev
            waits.append((sem, prev))
        self.dcnt[q][i] += 16
        tok = _Tok(sem, self.dcnt[q][i], "dma")
        self.ops[q].append(_Rec(waits, lambda e: e.dma_start(out=out, in_=in_, **kw), (sem, 16), True))
        self._record(tok, r, w)
        return tok

    def dma_fn(self, q, fn, r=(), w=()):
        if q in self.pending and self.pending[q]:
            self._resolve(self.pending[q][0])
        i = self.dnext[q]
        self.dnext[q] = (i + 1) % self.RING
        sem = self.dsem[q][i]
        waits = self._deps(q, r, w)
        prev = self.dcnt[q][i]
        if prev > 0 and self.seen[q].get(sem, 0) < prev:
            self.seen[q][sem] = prev
            waits.append((sem, prev))
        self.dcnt[q][i] += 16
        tok = _Tok(sem, self.dcnt[q][i], "dma")
        self.ops[q].append(_Rec(waits, fn, (sem, 16), True))
        self._record(tok, r, w)
        return tok

    def coll(self, fn, r=(), w=()):
        q = "pool"
        if self.pending[q]:
            self._resolve(self.pending[q][0])
        waits = self._deps(q, r, w)
        self.ccnt += 1
        tok = _Tok(self.csem, self.ccnt, "dma")
        self.ops[q].append(_Rec(waits, fn, (self.csem, 1), True))
        self._record(tok, r, w)
        return tok

    def bcreg(self, eng):
        if self._bc is None:
            self._bc = eng.to_reg(1023)
        return self._bc

    def flush(self):
        nc = self.nc
        self._bc = None
        for q in self.dsem:
            waits = []
            for i, sem in enumerate(self.dsem[q]):
                v = self.dcnt[q][i]
                if v > 0 and self.seen[q].get(sem, 0) < v:
                    self.seen[q][sem] = v
                    waits.append((sem, v))
            if waits:
                self.ops[q].append(_Rec(waits, None))
        if not any(self.ops.values()):
            self._reset()
            return
        with nc.Block() as block:
            decos = {"sp": block.sync, "act": block.scalar, "dve": block.vector,
                     "pool": block.gpsimd, "pe": block.tensor}
            for name in ("sp", "pool", "act", "dve", "pe"):
                ops = self.ops[name]
                if not ops:
                    continue

                def body(e, ops=ops):
                    for rec in ops:
                        for sem, v in rec.waits:
                            e.wait_ge(sem, v)
                        if rec.fn is not None:
                            ins = rec.fn(e)
                            if rec.inc is not None:
                                ins.then_inc(rec.inc[0], rec.inc[1])

                decos[name](body)
        self._reset()

    def mm(self, out, lhsT, rhs, start, stop, r=(), w=()):
        return self.op("pe", lambda e: e.matmul(out, lhsT=lhsT, rhs=rhs, start=start, stop=stop), r, w)

    def act(self, out, in_, func, r=(), w=(), **kw):
        return self.op("act", lambda e: e.activation(out=out, in_=in_, func=func, **kw), r, w)

    def ts(self, eng, out, in0, s1, s2, op0, op1=None, r=(), w=(), **kw):
        if op1 is None:
            return self.op(eng, lambda e: e.tensor_scalar(out=out, in0=in0, scalar1=s1, scalar2=None, op0=op0, **kw), r, w)
        return self.op(eng, lambda e: e.tensor_scalar(out=out, in0=in0, scalar1=s1, scalar2=s2, op0=op0, op1=op1, **kw), r, w)

    def tt(self, eng, out, in0, in1, op, r=(), w=()):
        return self.op(eng, lambda e: e.tensor_tensor(out=out, in0=in0, in1=in1, op=op), r, w)

    def stt(self, eng, out, in0, scalar, in1, op0, op1, r=(), w=()):
        return self.op(eng, lambda e: e.scalar_tensor_tensor(out=out, in0=in0, scalar=scalar, in1=in1, op0=op0, op1=op1), r, w)

    def cp(self, eng, out, in_, r=(), w=()):
        return self.op(eng, lambda e: e.tensor_copy(out=out, in_=in_), r, w)

    def memset(self, eng, ap, val, w=()):
        return self.op(eng, lambda e: e.memset(ap, val), (), w)


def _t5_bucket(rel):
    nb = 16
    ret = np.where(rel > 0, nb, 0)
    n = np.abs(rel)
    me = 8
    nf = np.maximum(n, 1).astype(np.float32)
    large = me + (np.log(nf / me) / np.float32(math.log(1024 / me)) * (nb - me)).astype(np.int32)
    large = np.minimum(large, nb - 1)
    return ret + np.where(n < me, n, large)


def _consts(core):
    half = core % 2
    c = {}
    eye = np.eye(128, dtype=np.float32)
    c["ident"] = eye
    c["antiid"] = eye[::-1].copy()
    c["ones"] = np.ones((128, 128), np.float32)
    p = np.arange(128)
    c["upper"] = (p[:, None] < p[None, :]).astype(np.float32)
    c["pairg"] = ((p[:, None] // 32 == p[None, :] // 32) & (p[:, None] % 16 == p[None, :] % 16)).astype(np.float32)
    sel = np.zeros((128, 16), np.float32)
    sel[core * 16 + np.arange(16), np.arange(16)] = 1.0
    c["sel"] = sel
    inv_freq = (10000.0 ** (-np.arange(0, 64, 2, dtype=np.float32) / 64)).astype(np.float32)
    col = np.zeros((128, 4), np.float32)
    col[:, 0] = np.tile(inv_freq, 4)
    col[:, 1] = EPS
    c["cols"] = col
    oh = np.zeros((32, 3, 384), np.float32)
    ng = np.zeros((128, 3, 3), np.float32)
    for g, (win, dd) in enumerate(DIL):
        for i in range(384):
            delta = 191 - i
            if abs(delta) <= 64 and i < 383:
                rel = delta * dd
                if half:
                    rel = -rel
                oh[int(_t5_bucket(np.array(rel))), g, i] = 1.0
            else:
                ng[i % 128, g, i // 128] = NEG * math.sqrt(128.0)
    c["oh"] = oh
    c["ng"] = ng
    return c


def build(debug=False, upto=99, ncores=NCORES):
    nc = bass.Bass("TRN2", target_bir_lowering=False)
    kb = KB(nc)

    def din(name, shape, dt=F32):
        return nc.dram_tensor(name, list(shape), dt, kind="ExternalInput")

    def dscr(name, shape, dt=F32, dbg=False):
        return nc.dram_tensor(name, list(shape), dt, kind="ExternalOutput" if (debug and dbg) else "Internal")

    xT = din("xT", [D, S])
    xo = din("xo", [NO, D])
    pos = din("pos", [1, S], I32)
    ccol = din("ccol", [128, 8])
    w_ada = din("w_ada", [D, 6 * D])
    b_ada = din("b_ada", [1, 6 * D])
    g_mix = din("g_mix", [1, D])
    w_in = din("w_in", [D, 11968])
    g_q = din("g_q", [1, 384])
    g_kv = din("g_kv", [1, 256])
    w_uq = din("w_uq", [384, 1536])
    w_ukv = din("w_ukv", [256, 2048])
    rel_bias = din("rel_bias", [32, 24])
    w_out = din("w_out", [D, D])
    g_ffn = din("g_ffn", [1, D])
    w_router = din("w_router", [D, 16])
    w_gate = din("w_gate", [16, D, 2048])
    w_up = din("w_up", [16, D, 2048])
    w_down = din("w_down", [16, 2048, D])
    g_final = din("g_final", [1, D])
    c_ident = din("c_ident", [128, 128])
    c_antiid = din("c_antiid", [128, 128])
    c_ones = din("c_ones", [128, 128])
    c_upper = din("c_upper", [128, 128])
    c_pairg = din("c_pairg", [128, 128])
    c_sel = din("c_sel", [128, 16])
    c_cols = din("c_cols", [128, 4])
    c_oh = din("c_oh", [32, 3, 384])
    c_ng = din("c_ng", [128, 3, 3])
    out_d = nc.dram_tensor("out", [NO, D], F32, kind="ExternalOutput")

    hT_d = dscr("hT_d", [D, 5120], BF16, True)
    ckvn_d = dscr("ckvn_d", [256, S], BF16, True)
    kpe_d = dscr("kpe_d", [64, S], BF16, True)
    qn_d = dscr("qn_d", [8, 128, NO], BF16, True)
    qpe_d = dscr("qpe_d", [8, 64, NO], BF16, True)
    modr_d = dscr("modr_d", [128, 4096], F32, True)
    ga_d = dscr("ga_d", [D, NO], BF16, True)
    mixb_d = dscr("mixb_d", [D, NO], BF16, True)
    mix_d = dscr("mix_d", [D, NO], BF16, True)
    wvec_d = dscr("wvec_d", [3, 8, 384], BF16, True)
    x1_d = dscr("x1_d", [NO, D], F32, True)
    afft_d = dscr("afft_d", [16, NO], F32)
    affall_d = dscr("affall_d", [ncores * 16, NO], F32)
    xe_l = [dscr(f"xe_d{i}", [1024, D], BF16) for i in range(16)]
    ye_l = [dscr(f"ye_d{i}", [1024, D], F32) for i in range(16)]
    dbg_d = dscr("dbg_d", [128, 2048], F32, True)

    gsb = lambda name, shape, dt=F32: nc.alloc_sbuf_tensor(name, list(shape), dt)
    ident_bf = gsb("ident_bf", [128, 128], BF16)
    antiid_bf = gsb("antiid_bf", [128, 128], BF16)
    ones_bf = gsb("ones_bf", [128, 128], BF16)
    upper_bf = gsb("upper_bf", [128, 128], BF16)
    ident_f = gsb("ident_f", [128, 128])
    pairg_f = gsb("pairg_f", [128, 128])
    sel_f = gsb("sel_f", [128, 16])
    cols = gsb("cols", [128, 4])
    A1 = gsb("A1", [128, 8])
    sh1c = gsb("sh1c", [128, 8])
    gqc = gsb("gqc", [128, 3])
    gkvc = gsb("gkvc", [128, 2])
    PS = lambda i: ("ps", i)

    invf = cols[:, 0:1]
    epsc = cols[:, 1:2]

    with ExitStack() as st:
        sb = lambda name, shape, dt=F32: st.enter_context(nc.sbuf_tensor(name, list(shape), dt))
        ps = [st.enter_context(nc.psum_tensor(f"ps0_{i}", [128, 512], F32)) for i in range(8)]
        for (dst, src) in ((ident_bf, c_ident), (antiid_bf, c_antiid), (ones_bf, c_ones), (upper_bf, c_upper),
                           (ident_f, c_ident), (pairg_f, c_pairg), (sel_f, c_sel), (cols, c_cols)):
            kb.dma("pool", dst[:], src.ap(), w=[dst.name])
        gmc = sb("gmc", [128, 8])
        kb.dma("sp", gmc[:], g_mix.ap().rearrange("o (j p) -> p (o j)", p=128), w=["gmc"], allow_slow_non_contiguous=True)
        kb.dma("sp", gqc[:], g_q.ap().rearrange("o (j p) -> p (o j)", p=128), w=["gqc"], allow_slow_non_contiguous=True)
        kb.dma("sp", gkvc[:], g_kv.ap().rearrange("o (j p) -> p (o j)", p=128), w=["gkvc"], allow_slow_non_contiguous=True)
        cc = sb("cc", [128, 8])
        kb.dma("sp", cc[:], ccol.ap(), w=["cc"])
        cond = sb("cond", [128, 8])
        kb.act(cond[:], cc[:], AF.Silu, r=["cc"], w=["cond"])
        condB = sb("condB", [128, 8, 128])
        kb.cp("dve", condB[:], cond[:].unsqueeze(2).to_broadcast([128, 8, 128]), r=["cond"], w=["condB"])
        badc = sb("badc", [128, 16])
        kb.dma("sp", badc[:], b_ada.ap()[:, 0:2048].rearrange("o (j p) -> p (o j)", p=128), w=["badc"], allow_slow_non_contiguous=True)
        badr = sb("badr", [128, 4096])
        kb.dma("sp", badr[:], bass.AP(b_ada, 2048, [[0, 128], [1, 4096]]), w=["badr"])
        wa = [sb(f"wa{i}", [128, 8, 512]) for i in range(2)]
        wav = w_ada.ap().rearrange("(j p) n -> p j n", p=128)
        modr = sb("modr", [128, 4096])
        modc = sb("modc", [128, 16])
        for blk in range(12):
            wb = wa[blk % 2]
            kb.dma("sp", wb[:], wav[:, :, blk * 512:(blk + 1) * 512], w=[("wa", blk % 2)])
            if blk < 4:
                for jj in range(4):
                    j = blk * 4 + jj
                    for kc in range(8):
                        kb.mm(ps[0][:, j:j + 1], wb[:, kc, jj * 128:(jj + 1) * 128], cond[:, kc:kc + 1], kc == 0, kc == 7,
                              r=[("wa", blk % 2), "cond"], w=[PS(0)])
            else:
                pb = 1 + (blk % 2)
                for kc in range(8):
                    kb.mm(ps[pb][:], condB[:, kc, :], wb[:, kc, :], kc == 0, kc == 7,
                          r=[("wa", blk % 2), "condB"], w=[PS(pb)])
                c0 = (blk - 4) * 512
                kb.tt("dve", modr[:, c0:c0 + 512], ps[pb][:], badr[:, c0:c0 + 512], ALU.add,
                      r=[PS(pb), "badr"], w=["modr"])
        kb.tt("dve", modc[:], ps[0][:, 0:16], badc[:], ALU.add, r=[PS(0), "badc"], w=["modc"])
        kb.ts("dve", A1[:], modc[:, 8:16], 1.0, None, ALU.add, r=["modc"], w=["A1"])
        kb.tt("dve", A1[:], A1[:], gmc[:], ALU.mult, r=["A1", "gmc"], w=["A1"])
        kb.cp("dve", sh1c[:], modc[:, 0:8], r=["modc"], w=["sh1c"])
        kb.dma("sp", modr_d.ap(), modr[:], r=["modr"])
        zt = sb("zt", [128, 8, 1024], BF16)
        kb.memset("pool", zt[:], 0.0, w=["zt"])
        for e_ in range(16):
            kb.dma("act", xe_l[e_].ap().rearrange("(a p) d -> p a d", p=128), zt[:], r=["zt"])
        kb.flush()
    if upto <= 0:
        return nc

    with ExitStack() as st:
        sb = lambda name, shape, dt=F32: st.enter_context(nc.sbuf_tensor(name, list(shape), dt))
        ps = [st.enter_context(nc.psum_tensor(f"ps1_{i}", [128, 512], F32)) for i in range(8)]
        wkv = sb("wkv", [128, 8, 384], BF16)
        wiv = w_in.ap().rearrange("(j p) n -> p j n", p=128)
        kb.dma("pool", wkv[:, :, 0:320], wiv[:, :, 384:704], w=["wkv"])
        kb.dma("pool", wkv[:, :, 320:352], wiv[:, :, 672:704], w=["wkv"])
        kb.dma("pool", wkv[:, :, 352:384], wiv[:, :, 640:672], w=["wkv"])
        kb.ts("dve", wkv[:, :, 320:352], wkv[:, :, 320:352], -1.0, None, ALU.mult, r=["wkv"], w=["wkv"])
        wq = sb("wq", [128, 8, 384], BF16)
        kb.dma("pool", wq[:], wiv[:, :, 0:384], w=["wq"])
        wuq = sb("wuq", [128, 3, 1536], BF16)
        kb.dma("pool", wuq[:], w_uq.ap().rearrange("(r p) n -> p r n", p=128), w=["wuq"])
        wuqr = sb("wuqr", [128, 3, 8, 64], BF16)
        wuqv = w_uq.ap().rearrange("(r p) (h c) -> p r h c", p=128, c=192)
        for rr in range(3):
            kb.dma("pool", wuqr[:, rr, :, 0:32], wuqv[:, rr, :, 160:192], w=["wuqr"])
            kb.dma("pool", wuqr[:, rr, :, 32:64], wuqv[:, rr, :, 128:160], w=["wuqr"])
        kb.ts("dve", wuqr[:, :, :, 0:32], wuqr[:, :, :, 0:32], -1.0, None, ALU.mult, r=["wuqr"], w=["wuqr"])

        xbuf = [sb(f"xc{i}", [128, 8, 512]) for i in range(2)]
        sqb = sb("sqb", [128, 8, 512], BF16)
        rstd = sb("rstd", [128, 512])
        t1 = [sb(f"t1_{i}", [128, 512]) for i in range(2)]
        hc = [sb(f"hc{i}", [128, 8, 512], BF16) for i in range(2)]
        sq2 = sb("sq2", [128, 3, 512], BF16)
        rstd2 = sb("rstd2", [128, 512])
        ckvn = sb("ckvn", [128, 2, 512], BF16)
        posi = sb("posi", [64, 512], I32)
        ang = sb("ang", [64, 512])
        angk = sb("angk", [64, 512])
        angi = sb("angi", [64, 512], I32)
        sn = sb("sn", [64, 512])
        cs = sb("cs", [64, 512])
        r1 = sb("r1", [64, 512])
        r2 = sb("r2", [64, 512])
        kper = sb("kper", [64, 512], BF16)
        cqn = sb("cqn", [128, 3, 512], BF16)
        qns = [sb(f"qns{i}", [128, 512], BF16) for i in range(2)]
        qps = [sb(f"qps{i}", [64, 512], BF16) for i in range(2)]
        xTv = xT.ap().rearrange("(j p) t -> p j t", p=128)
        hTv = hT_d.ap().rearrange("(j p) t -> p j t", p=128)
        ckv_v = ckvn_d.ap().rearrange("(r p) t -> p r t", p=128)

        def sincos(t, dst, shift, tag):
            src = ang
            if shift != 0.0:
                kb.ts("dve", angk[:], ang[:], shift, None, ALU.add, r=["ang"], w=["angk"])
                src = angk
            kb.ts("dve", r1[:], src[:], 1.0 / (2 * math.pi), None, ALU.mult, r=["ang", "angk"], w=["r1"])
            kb.cp("dve", angi[:], r1[:], r=["r1"], w=["angi"])
            kb.cp("dve", r1[:], angi[:], r=["angi"], w=["r1"])
            kb.stt("dve", r2[:], r1[:], -TWO_PI_HI, src[:], ALU.mult, ALU.add, r=["r1", "ang", "angk"], w=["r2"])
            kb.stt("dve", r2[:], r1[:], -TWO_PI_LO, r2[:], ALU.mult, ALU.add, r=["r1", "r2"], w=["r2"])
            kb.ts("dve", r2[:], r2[:], -3.1415925, 3.1415925, ALU.max, ALU.min, r=["r2"], w=["r2"])
            kb.act(dst[:], r2[:], AF.Sin, r=["r2"], w=[tag])

        for t in range(16):
            xc = xbuf[t % 2]
            XK = ("xc", t % 2)
            h_ = hc[t % 2]
            HK = ("hc", t % 2)
            tsl = slice(t * 512, (t + 1) * 512)
            kb.dma("sp", xc[:], xTv[:, :, tsl], w=[XK])
            kb.dma("act", posi[:], bass.AP(pos, t * 512, [[0, 64], [1, 512]]), w=["posi"])
            kb.act(sqb[:], xc[:], AF.Square, r=[XK], w=["sqb"])
            for j in range(8):
                kb.mm(ps[0][:], ones_bf[:], sqb[:, j, :], j == 0, j == 7, r=["sqb"], w=[PS(0)])
            kb.act(rstd[:], ps[0][:], AF.Sqrt, bias=epsc, scale=1.0 / D, r=[PS(0)], w=["rstd"])
            kb.op("dve", lambda e: e.reciprocal(out=rstd[:], in_=rstd[:]), r=["rstd"], w=["rstd"])
            for j in range(8):
                tt_ = t1[j % 2]
                kb.stt("dve", tt_[:], xc[:, j, :], A1[:, j:j + 1], rstd[:], ALU.mult, ALU.mult,
                       r=[XK, "rstd"], w=[("t1", j % 2)])
                kb.act(h_[:, j, :], tt_[:], AF.Identity, bias=sh1c[:, j:j + 1], scale=1.0, r=[("t1", j % 2)], w=[HK])
            if t < 10:
                kb.dma("sp", hTv[:, :, tsl], h_[:], r=[HK])
            for rr in range(2):
                for j in range(8):
                    kb.mm(ps[1 + rr][:], wkv[:, j, rr * 128:(rr + 1) * 128], h_[:, j, :], j == 0, j == 7,
                          r=["wkv", HK], w=[PS(1 + rr)])
            for j in range(8):
                kb.mm(ps[3][0:64, :], wkv[:, j, 256:320], h_[:, j, :], j == 0, j == 7, r=["wkv", HK], w=[PS(3)])
            for j in range(8):
                kb.mm(ps[4][0:64, :], wkv[:, j, 320:384], h_[:, j, :], j == 0, j == 7, r=["wkv", HK], w=[PS(4)])
            for rr in range(2):
                kb.act(sq2[:, rr, :], ps[1 + rr][:], AF.Square, r=[PS(1 + rr)], w=["sq2"])
            for rr in range(2):
                kb.mm(ps[5][:], ones_bf[:], sq2[:, rr, :], rr == 0, rr == 1, r=["sq2"], w=[PS(5)])
            kb.act(rstd2[:], ps[5][:], AF.Sqrt, bias=epsc, scale=1.0 / 256, r=[PS(5)], w=["rstd2"])
            kb.op("dve", lambda e: e.reciprocal(out=rstd2[:], in_=rstd2[:]), r=["rstd2"], w=["rstd2"])
            for rr in range(2):
                kb.stt("dve", ckvn[:, rr, :], ps[1 + rr][:], gkvc[:, rr:rr + 1], rstd2[:], ALU.mult, ALU.mult,
                       r=[PS(1 + rr), "rstd2"], w=["ckvn"])
            kb.dma("sp", ckv_v[:, :, tsl], ckvn[:], r=["ckvn"])
            kb.cp("dve", ang[:], posi[:], r=["posi"], w=["ang"])
            kb.ts("dve", ang[:], ang[:], invf[0:64, :], None, ALU.mult, r=["ang"], w=["ang"])
            sincos(t, sn, 0.0, "sn")
            sincos(t, cs, math.pi / 2, "cs")
            kb.tt("dve", r1[:], ps[3][0:64, :], cs[:], ALU.mult, r=[PS(3), "cs"], w=["r1"])
            kb.tt("dve", r2[:], ps[4][0:64, :], sn[:], ALU.mult, r=[PS(4), "sn"], w=["r2"])
            kb.tt("dve", kper[:], r1[:], r2[:], ALU.add, r=["r1", "r2"], w=["kper"])
            kb.dma("sp", kpe_d.ap()[:, tsl], kper[:], r=["kper"])
            if t >= 8:
                continue
            for rr in range(3):
                for j in range(8):
                    kb.mm(ps[1 + rr][:], wq[:, j, rr * 128:(rr + 1) * 128], h_[:, j, :], j == 0, j == 7,
                          r=["wq", HK], w=[PS(1 + rr)])
            for rr in range(3):
                kb.act(sq2[:, rr, :], ps[1 + rr][:], AF.Square, r=[PS(1 + rr)], w=["sq2"])
            for rr in range(3):
                kb.mm(ps[5][:], ones_bf[:], sq2[:, rr, :], rr == 0, rr == 2, r=["sq2"], w=[PS(5)])
            kb.act(rstd2[:], ps[5][:], AF.Sqrt, bias=epsc, scale=1.0 / 384, r=[PS(5)], w=["rstd2"])
            kb.op("dve", lambda e: e.reciprocal(out=rstd2[:], in_=rstd2[:]), r=["rstd2"], w=["rstd2"])
            for rr in range(3):
                kb.stt("dve", cqn[:, rr, :], ps[1 + rr][:], gqc[:, rr:rr + 1], rstd2[:], ALU.mult, ALU.mult,
                       r=[PS(1 + rr), "rstd2"], w=["cqn"])
            for h in range(8):
                pn, pp, pr = (0, 1, 2) if h % 2 == 0 else (3, 4, 6)
                for rr in range(3):
                    kb.mm(ps[pn][:], wuq[:, rr, h * 192:h * 192 + 128], cqn[:, rr, :], rr == 0, rr == 2,
                          r=["wuq", "cqn"], w=[PS(pn)])
                for rr in range(3):
                    kb.mm(ps[pp][0:64, :], wuq[:, rr, h * 192 + 128:h * 192 + 192], cqn[:, rr, :], rr == 0, rr == 2,
                          r=["wuq", "cqn"], w=[PS(pp)])
                for rr in range(3):
                    kb.mm(ps[pr][0:64, :], wuqr[:, rr, h, :], cqn[:, rr, :], rr == 0, rr == 2,
                          r=["wuqr", "cqn"], w=[PS(pr)])
                qn_ = qns[h % 2]
                qp_ = qps[h % 2]
                kb.act(qn_[:], ps[pn][:], AF.Copy, r=[PS(pn)], w=[("qns", h % 2)])
                kb.tt("dve", r1[:], ps[pp][0:64, :], cs[:], ALU.mult, r=[PS(pp), "cs"], w=["r1"])
                kb.tt("dve", r2[:], ps[pr][0:64, :], sn[:], ALU.mult, r=[PS(pr), "sn"], w=["r2"])
                kb.tt("dve", qp_[:], r1[:], r2[:], ALU.add, r=["r1", "r2"], w=[("qps", h % 2)])
                kb.dma("sp", qn_d.ap()[h, :, tsl], qn_[:], r=[("qns", h % 2)])
                kb.dma("sp", qpe_d.ap()[h, :, tsl], qp_[:], r=[("qps", h % 2)])
        kb.flush()
    if upto <= 1:
        return nc
    GCOL = 704 + 9216
    wiv = w_in.ap().rearrange("(j p) n -> p j n", p=128)
    with ExitStack() as st:
        sb = lambda name, shape, dt=F32: st.enter_context(nc.sbuf_tensor(name, list(shape), dt))
        ps = [st.enter_context(nc.psum_tensor(f"ps3_{i}", [128, 512], F32)) for i in range(8)]
        tabf = sb("tabf", [32, 24])
        tabb = sb("tabb", [32, 24], BF16)
        ohb = sb("ohb", [32, 3, 384], BF16)
        ngs = sb("ngs", [128, 3, 3])
        wvs = sb("wvs", [128, 9, 8], BF16)
        kb.dma("sp", tabf[:], rel_bias.ap(), w=["tabf"])
        kb.cp("dve", tabb[:], tabf[:], r=["tabf"], w=["tabb"])
        kb.dma("pool", ohb[:], c_oh.ap(), w=["ohb"])
        kb.dma("sp", ngs[:], c_ng.ap(), w=["ngs"])
        for g in range(3):
            for ic in range(3):
                kb.mm(ps[0][:, (g * 3 + ic) * 8:(g * 3 + ic) * 8 + 8], ohb[:, g, ic * 128:(ic + 1) * 128],
                      tabb[:, g * 8:(g + 1) * 8], True, True, r=["ohb", "tabb"], w=[PS(0)])
        for g in range(3):
            for ic in range(3):
                k9 = g * 3 + ic
                kb.ts("dve", wvs[:, k9, :], ps[0][:, k9 * 8:k9 * 8 + 8], math.sqrt(128.0), ngs[:, g, ic:ic + 1],
                      ALU.mult, ALU.add, r=[PS(0), "ngs"], w=["wvs"])
                kb.dma("sp", wvec_d.ap()[g, :, ic * 128:(ic + 1) * 128].rearrange("h i -> i h"), wvs[:, k9, :],
                       r=["wvs"], w=["wvec_d"], allow_slow_non_contiguous=True)
        kb.flush()

        hT = sb("hT", [128, 8, 5120], BF16)
        hTv = hT_d.ap().rearrange("(j p) t -> p j t", p=128)
        for t in range(10):
            kb.dma("sp" if t % 2 == 0 else "act", hT[:, :, t * 512:(t + 1) * 512],
                   hTv[:, :, t * 512:(t + 1) * 512], w=[("hT", t)])
        HTK = [("hT", t) for t in range(10)]
        wd = [[sb(f"wd{s_}_{t_}", [128, 8, 128], BF16) for t_ in range(3)] for s_ in range(2)]
        wg = [sb(f"wg{t_}", [128, 8, 128], BF16) for t_ in range(2)]
        Oacc = sb("Oacc", [128, NO])
        Dacc = sb("Dacc", [128, NO])
        HM = [[sb(f"HM{s_}_{t_}", [128, 4, 128], BF16) for t_ in range(3)] for s_ in range(2)]
        qc_t = sb("qc_t", [128, NO], BF16)
        kc_t = sb("kc_t", [128, NO + 128 * 16], BF16)
        vTn = sb("vTn", [128, 64 * 16 + NO + 64 * 16], BF16)
        Vtm = sb("Vtm", [128, 48, 128], BF16)
        PA = sb("PA", [128, 512], BF16)
        PB_ = sb("PB_", [128, 512], BF16)
        gst = sb("gst", [128, 512])
        rcp = sb("rcp", [128, 512])
        ost = [sb(f"ost{i}", [128, 512], BF16) for i in range(2)]
        SC = 128.0 ** -0.5
        unit = 0
        for h in range(8):
            for g, (win, dd) in enumerate(DIL):
                slot = unit % 2
                unit += 1
                WK = ("wd", slot)
                for t_ in range(3):
                    c0 = 704 + ((g * 3 + t_) * 8 + h) * 128
                    kb.dma("pool", wd[slot][t_][:], wiv[:, :, c0:c0 + 128], w=[WK])
                wq_, wk_, wv_ = wd[slot]
                HK_ = ("HM", slot)
                base = (g * 8 + h) * 384
                kb.dma("sp", HM[slot][0][:], bass.AP(wvec_d, base, [[1, 128], [0, 4], [1, 128]]), r=["wvec_d"], w=[HK_])
                kb.dma("sp", HM[slot][1][:], bass.AP(wvec_d, base + 128, [[1, 128], [0, 4], [1, 128]]), r=["wvec_d"], w=[HK_])
                kb.dma("sp", HM[slot][2][:], bass.AP(wvec_d, base + 128, [[1, 128], [0, 4], [1, 128]]), r=["wvec_d"], w=[HK_])
                kb.memset("pool", HM[slot][2][64:128, 0, :], NEG * math.sqrt(128.0), w=[HK_])
                HB, HA, HA0 = HM[slot]
                Lh = NO // dd
                LK = Lh + 128
                J = Lh // 128 + 1
                TK = NO + 64 * dd
                n = min(4, Lh // 128)
                N = 128 * n
                qcv = qc_t[:, 0:NO].rearrange("p (c l) -> p c l", c=dd)
                kcv = kc_t[:, 0:dd * LK].rearrange("p (c l) -> p c l", c=dd)
                kb.memset("pool", kcv[:, :, 0:64], 0.0, w=["kc"])
                kb.memset("pool", vTn[:, 0:64 * dd], 0.0, w=["vTn"])
                nblk = (TK + 511) // 512
                for t in range(nblk):
                    nt = min(512, TK - 512 * t)
                    tsl = slice(512 * t, 512 * t + nt)
                    la, lb = 512 * t // dd, (512 * t + nt) // dd
                    if t < 8:
                        for kc in range(8):
                            kb.mm(ps[0][:], wq_[:, kc, :], hT[:, kc, tsl], kc == 0, kc == 7, r=[WK, ("hT", t)], w=[PS(0)])
                        kb.act(qcv[:, :, la:lb].rearrange("p c l -> p l c"), ps[0][:].rearrange("p (l c) -> p l c", c=dd), AF.Copy,
                               r=[PS(0)], w=["qc"])
                    for kc in range(8):
                        kb.mm(ps[1][:, 0:nt], wk_[:, kc, :], hT[:, kc, tsl], kc == 0, kc == 7, r=[WK, ("hT", t)], w=[PS(1)])
                    kb.cp("dve", kcv[:, :, 64 + la:64 + lb].rearrange("p c l -> p l c"),
                          ps[1][:, 0:nt].rearrange("p (l c) -> p l c", c=dd), r=[PS(1)], w=["kc"])
                    for kc in range(8):
                        kb.mm(ps[2][:, 0:nt], wv_[:, kc, :], hT[:, kc, tsl], kc == 0, kc == 7, r=[WK, ("hT", t)], w=[PS(2)])
                    kb.act(vTn[:, 64 * dd + 512 * t:64 * dd + 512 * t + nt], ps[2][:, 0:nt], AF.Copy, r=[PS(2)], w=["vTn"])
                ntile = dd * J
                for t4 in range((ntile + 3) // 4):
                    k4 = min(4, ntile - 4 * t4)
                    for q_ in range(k4):
                        ti = 4 * t4 + q_
                        c, j = ti // J, ti % J
                        a0 = 128 * j * dd + c
                        kb.mm(ps[3][:, q_ * 128:(q_ + 1) * 128], vTn[:, SS(a0, 128, dd)], ident_bf[:], True, True,
                              r=["vTn"], w=[PS(3)])
                    if t4 % 2 == 0:
                        kb.act(Vtm[:, 4 * t4:4 * t4 + k4, :], ps[3][:, 0:k4 * 128].rearrange("p (j v) -> p j v", v=128), AF.Copy,
                               r=[PS(3)], w=["Vtm"])
                    else:
                        kb.cp("dve", Vtm[:, 4 * t4:4 * t4 + k4, :], ps[3][:, 0:k4 * 128].rearrange("p (j v) -> p j v", v=128),
                              r=[PS(3)], w=["Vtm"])
                for c in range(dd):
                    for mb in range(Lh // N):
                        l0 = mb * N
                        hA = HA0 if l0 == 0 else HA
                        kb.mm(ps[4][:, 0:N], antiid_bf[:], hA[:, 0:n, :], True, False, r=[HK_], w=[PS(4)])
                        for i in range(n):
                            kb.mm(ps[4][:, i * 128:(i + 1) * 128], kcv[:, c, l0 + i * 128:l0 + (i + 1) * 128],
                                  qcv[:, c, l0 + i * 128:l0 + (i + 1) * 128], False, i == n - 1, r=["kc", "qc"], w=[PS(4)])
                        kb.mm(ps[5][:, 0:N], antiid_bf[:], HB[:, 0:n, :], True, False, r=[HK_], w=[PS(5)])
                        for i in range(n):
                            kb.mm(ps[5][:, i * 128:(i + 1) * 128], kcv[:, c, l0 + (i + 1) * 128:l0 + (i + 2) * 128],
                                  qcv[:, c, l0 + i * 128:l0 + (i + 1) * 128], False, i == n - 1, r=["kc", "qc"], w=[PS(5)])
                        kb.act(PA[:, 0:N], ps[4][:, 0:N], AF.Exp, scale=SC, r=[PS(4)], w=["PA"])
                        kb.act(PB_[:, 0:N], ps[5][:, 0:N], AF.Exp, scale=SC, r=[PS(5)], w=["PB"])
                        for i in range(n):
                            vt = c * J + mb * n + i
                            kb.mm(ps[6][:, i * 128:(i + 1) * 128], Vtm[:, vt, :], PA[:, i * 128:(i + 1) * 128], True, False,
                                  r=["Vtm", "PA"], w=[PS(6)])
                            kb.mm(ps[6][:, i * 128:(i + 1) * 128], Vtm[:, vt + 1, :], PB_[:, i * 128:(i + 1) * 128], False, True,
                                  r=["Vtm", "PB"], w=[PS(6)])
                        kb.mm(ps[7][:, 0:N], ones_bf[:], PA[:, 0:N], True, False, r=["PA"], w=[PS(7)])
                        kb.mm(ps[7][:, 0:N], ones_bf[:], PB_[:, 0:N], False, True, r=["PB"], w=[PS(7)])
                        u0 = l0 * dd + c
                        ov = Oacc[:, SS(u0, N, dd)]
                        dv = Dacc[:, SS(u0, N, dd)]
                        if g == 0:
                            kb.cp("dve", ov, ps[6][:, 0:N], r=[PS(6)], w=["Oacc"])
                            kb.cp("dve", dv, ps[7][:, 0:N], r=[PS(7)], w=["Dacc"])
                        else:
                            kb.tt("dve", ov, ov, ps[6][:, 0:N], ALU.add, r=[PS(6), "Oacc"], w=["Oacc"])
                            kb.tt("dve", dv, dv, ps[7][:, 0:N], ALU.add, r=[PS(7), "Dacc"], w=["Dacc"])
            kb.dma("pool", wg[0][:], wiv[:, :, GCOL + h * 128:GCOL + (h + 1) * 128], w=["wg0"])
            kb.dma("pool", wg[1][:], wiv[:, :, GCOL + D + h * 128:GCOL + D + (h + 1) * 128], w=["wg1"])
            for qc in range(8):
                tsl = slice(qc * 512, (qc + 1) * 512)
                hsl = slice(qc * 512, (qc + 1) * 512)
                for kc in range(8):
                    kb.mm(ps[0][:], wg[0][:, kc, :], hT[:, kc, hsl], kc == 0, kc == 7, r=["wg0", ("hT", qc)], w=[PS(0)])
                for kc in range(8):
                    kb.mm(ps[1][:], wg[1][:, kc, :], hT[:, kc, hsl], kc == 0, kc == 7, r=["wg1", ("hT", qc)], w=[PS(1)])
                o0, o1 = ost
                kb.act(o0[:], ps[0][:], AF.Sigmoid, r=[PS(0)], w=["ost0"])
                kb.dma("sp", ga_d.ap()[h * 128:(h + 1) * 128, tsl], o0[:], r=["ost0"])
                kb.act(gst[:], ps[1][:], AF.Sigmoid, r=[PS(1)], w=["gst"])
                kb.op("dve", lambda e, tsl=tsl: e.reciprocal(out=rcp[:], in_=Dacc[:, tsl]), r=["Dacc"], w=["rcp"])
                kb.tt("dve", rcp[:], rcp[:], Oacc[:, tsl], ALU.mult, r=["rcp", "Oacc"], w=["rcp"])
                kb.tt("dve", o1[:], rcp[:], gst[:], ALU.mult, r=["rcp", "gst"], w=["ost1"])
                kb.dma("sp", mixb_d.ap()[h * 128:(h + 1) * 128, tsl], o1[:], r=["ost1"])
        kb.flush()
    if upto <= 3:
        return nc

    with ExitStack() as st:
        sb = lambda name, shape, dt=F32: st.enter_context(nc.sbuf_tensor(name, list(shape), dt))
        ps = [st.enter_context(nc.psum_tensor(f"ps4_{i}", [128, 512], F32)) for i in range(8)]
        ckvn = sb("ckvn4", [128, 2, S], BF16)
        kpe = sb("kpe4", [128, S], BF16)
        ckv_v = ckvn_d.ap().rearrange("(r p) t -> p r t", p=128)
        for t in range(4):
            kb.dma("sp" if t % 2 == 0 else "act", ckvn[:, :, t * 2048:(t + 1) * 2048], ckv_v[:, :, t * 2048:(t + 1) * 2048], w=["ckvn"])
        kb.dma("sp", kpe[0:64, :], kpe_d.ap(), w=["kpe"])
        kb.dma("act", kpe[64:128, :], kpe_d.ap(), w=["kpe"])
        wukv = [sb(f"wukv{i}", [128, 2, 256], BF16) for i in range(2)]
        wukv_v = w_ukv.ap().rearrange("(r p) n -> p r n", p=128)
        KT = sb("KT", [128, S], BF16)
        V = sb("V", [128, 64, 128], BF16)
        Qb = [sb(f"Qb{i}", [128, NO], BF16) for i in range(2)]
        QPb = [sb(f"QPb{i}", [128, NO], BF16) for i in range(2)]
        Psum = [sb(f"Psum{i}", [128, 512], BF16) for i in range(3)]
        gab = [sb(f"gab{i}", [128, NO], BF16) for i in range(2)]
        mbb = [sb(f"mbb{i}", [128, NO], BF16) for i in range(2)]
        Pr = [sb(f"Pr{i}", [128, 512], BF16) for i in range(6)]
        accD = [sb(f"accD{i}", [128, 512]) for i in range(2)]
        accP = [sb(f"accP{i}", [128, 512]) for i in range(2)]
        ones_f = sb("ones_f", [128, 128])
        kb.dma("sp", ones_f[:], c_ones.ap(), w=["ones_f"])
        rcp = sb("rcp4", [128, 512])
        ot = sb("ot4", [128, 512])
        mst = [sb(f"mst{i}", [128, 512], BF16) for i in range(2)]
        SCA = 192.0 ** -0.5
        for h in range(8):
            s_ = h % 2
            kb.dma("pool", wukv[s_][:], wukv_v[:, :, h * 256:(h + 1) * 256], w=[("wukv", s_)])
            kb.dma("sp", Qb[s_][:], qn_d.ap()[h], w=[("Qb", s_)])
            kb.dma("sp", QPb[s_][0:64, :], qpe_d.ap()[h], w=[("QPb", s_)])
            kb.dma("sp", QPb[s_][64:128, :], qpe_d.ap()[h], w=[("QPb", s_)])
            kb.dma("act", gab[s_][:], ga_d.ap()[h * 128:(h + 1) * 128, :], w=[("gab", s_)])
            kb.dma("act", mbb[s_][:], mixb_d.ap()[h * 128:(h + 1) * 128, :], w=[("mbb", s_)])
            wu = wukv[s_]
            for tc in range(16):
                pb = tc % 2
                for rr in range(2):
                    kb.mm(ps[pb][:], wu[:, rr, 0:128], ckvn[:, rr, tc * 512:(tc + 1) * 512], rr == 0, rr == 1,
                          r=[("wukv", s_), "ckvn"], w=[PS(pb)])
                if tc % 2 == 0:
                    kb.act(KT[:, tc * 512:(tc + 1) * 512], ps[pb][:], AF.Copy, r=[PS(pb)], w=["KT"])
                else:
                    kb.cp("dve", KT[:, tc * 512:(tc + 1) * 512], ps[pb][:], r=[PS(pb)], w=["KT"])
            for t4 in range(16):
                pb = 2 + t4 % 2
                for jj in range(4):
                    tt_ = t4 * 4 + jj
                    for rr in range(2):
                        kb.mm(ps[pb][:, jj * 128:(jj + 1) * 128], ckvn[:, rr, tt_ * 128:(tt_ + 1) * 128], wu[:, rr, 128:256],
                              rr == 0, rr == 1, r=[("wukv", s_), "ckvn"], w=[PS(pb)])
                vdst = V[:, t4 * 4:(t4 + 1) * 4, :]
                vsrc = ps[pb][:].rearrange("p (j v) -> p j v", v=128)
                if t4 % 2 == 0:
                    kb.act(vdst, vsrc, AF.Copy, r=[PS(pb)], w=["V"])
                else:
                    kb.cp("dve", vdst, vsrc, r=[PS(pb)], w=["V"])
            steps = [(qc, kt) for qc in range(8) for kt in range(64)]
            NST = len(steps)
            LAG = 3
            NSB = 5
            for p2 in range(NST // 2 + 2):
                if 2 * p2 < NST:
                    for i in (2 * p2, 2 * p2 + 1):
                        qc, kt = steps[i]
                        sbk = i % NSB
                        kb.mm(ps[sbk][:], KT[:, kt * 128:(kt + 1) * 128], Qb[s_][:, qc * 512:(qc + 1) * 512], True, False,
                              r=["KT", ("Qb", s_)], w=[PS(sbk)])
                    for i in (2 * p2, 2 * p2 + 1):
                        qc, kt = steps[i]
                        sbk = i % NSB
                        rg = (i % 2) * 64
                        kb.mm(ps[sbk][:], kpe[rg:rg + 64, kt * 128:(kt + 1) * 128], QPb[s_][rg:rg + 64, qc * 512:(qc + 1) * 512],
                              False, True, r=["kpe", ("QPb", s_)], w=[PS(sbk)])
                for j in (2 * p2 - LAG, 2 * p2 + 1 - LAG):
                    if j < 0 or j >= NST:
                        continue
                    qc, kt = steps[j]
                    sbk = j % NSB
                    pk = j % 6
                    P_ = Pr[pk]
                    kb.act(P_[:], ps[sbk][:], AF.Exp, scale=SCA, r=[PS(sbk)], w=[("Pr", pk)])
                    po, pd = 6 + qc % 2, 5
                    kb.mm(ps[po][:], V[:, kt, :], P_[:], kt == 0, kt == 63, r=["V", ("Pr", pk)], w=[PS(po)])
                    if kt % 2 == 1:
                        sk = (j // 2) % 3
                        Pp = Pr[(j - 1) % 6]
                        kb.tt("dve", Psum[sk][:], Pp[:], P_[:], ALU.add, r=[("Pr", (j - 1) % 6), ("Pr", pk)], w=[("Psum", sk)])
                        kb.mm(ps[pd][:], ones_bf[:], Psum[sk][:], kt == 1, kt == 63, r=[("Psum", sk)], w=[PS(pd)])
                    if kt == 63:
                        qsl = slice(qc * 512, (qc + 1) * 512)
                        m_ = mst[qc % 2]
                        kb.op("dve", lambda e, pd=pd: e.reciprocal(out=rcp[:], in_=ps[pd][:]), r=[PS(pd)], w=["rcp"])
                        kb.tt("dve", ot[:], ps[po][:], rcp[:], ALU.mult, r=[PS(po), "rcp"], w=["ot"])
                        kb.tt("dve", ot[:], ot[:], gab[s_][:, qsl], ALU.mult, r=["ot", ("gab", s_)], w=["ot"])
                        kb.tt("dve", m_[:], ot[:], mbb[s_][:, qsl], ALU.add, r=["ot", ("mbb", s_)], w=[("mst", qc % 2)])
                        kb.dma("sp", mix_d.ap()[h * 128:(h + 1) * 128, qsl], m_[:], r=[("mst", qc % 2)])
        kb.flush()
    if upto <= 4:
        return nc
    idx = gsb("idx", [128, 32, 16], I32)
    wgt = gsb("wgt", [128, 32, 16])
    with ExitStack() as st:
        sb = lambda name, shape, dt=F32: st.enter_context(nc.sbuf_tensor(name, list(shape), dt))
        ps = [st.enter_context(nc.psum_tensor(f"ps5_{i}", [128, 512], F32)) for i in range(8)]
        wout = sb("wout", [128, 8, D], BF16)
        kb.dma("pool", wout[:], w_out.ap().rearrange("(j p) n -> p j n", p=128), w=["wout"])
        wr = sb("wr", [128, 8, 16], BF16)
        kb.dma("pool", wr[:], w_router.ap().rearrange("(j p) n -> p j n", p=128), w=["wr"])
        mrow = sb("mrow", [128, 3, D])
        kb.dma("sp", mrow[:], modr_d.ap()[:, 0:3 * D].rearrange("p (a n) -> p a n", n=D), w=["mrow"])
        gfb = sb("gfb", [128, D])
        kb.dma("sp", gfb[:], bass.AP(g_ffn, 0, [[0, 128], [1, D]]), w=["gfb"])
        B2 = sb("B2", [128, D])
        kb.ts("dve", B2[:], mrow[:, 2, :], 1.0, None, ALU.add, r=["mrow"], w=["B2"])
        kb.tt("dve", B2[:], B2[:], gfb[:], ALU.mult, r=["B2", "gfb"], w=["B2"])
        h2res = sb("h2res", [128, 32, D], BF16)
        aff_own = sb("aff_own", [128, 32, 16])
        affT = sb("affT", [16, NO])
        mixT = [sb(f"mixT{i}", [128, 8, 512], BF16) for i in range(2)]
        xot = [sb(f"xot{i}", [128, D]) for i in range(2)]
        x1t = [sb(f"x1t{i}", [128, D]) for i in range(2)]
        junk = sb("junk5", [128, D], BF16)
        tmpf = sb("tmpf", [128, D])
        h2T = sb("h2T", [128, 8, 128], BF16)
        sml = sb("sml", [128, 8])
        ex = sb("ex", [128, 16])
        mixv = mix_d.ap().rearrange("(j p) t -> p j t", p=128)
        for ch in range(8):
            mT = mixT[ch % 2]
            MK = ("mixT", ch % 2)
            kb.dma("sp", mT[:], mixv[:, :, ch * 512:(ch + 1) * 512], w=[MK])
            for i in range(4):
                tt_ = ch * 4 + i
                xo_ = xot[tt_ % 2]
                x1_ = x1t[tt_ % 2]
                XO, X1 = ("xot", tt_ % 2), ("x1t", tt_ % 2)
                kb.dma("act", xo_[:], xo.ap()[tt_ * 128:(tt_ + 1) * 128, :], w=[XO])
                for hf in range(2):
                    for kc in range(8):
                        kb.mm(ps[hf][:], mT[:, kc, i * 128:(i + 1) * 128], wout[:, kc, hf * 512:(hf + 1) * 512], kc == 0, kc == 7,
                              r=[MK, "wout"], w=[PS(hf)])
                    kb.tt("dve", x1_[:, hf * 512:(hf + 1) * 512], ps[hf][:], mrow[:, 0, hf * 512:(hf + 1) * 512], ALU.mult,
                          r=[PS(hf), "mrow"], w=[X1])
                kb.tt("pool", x1_[:], x1_[:], xo_[:], ALU.add, r=[X1, XO], w=[X1])
                kb.dma("sp", x1_d.ap()[tt_ * 128:(tt_ + 1) * 128, :], x1_[:], r=[X1])
                kb.memset("dve", sml[:, 0:1], 0.0, w=["sml"])
                kb.act(junk[:], x1_[:], AF.Square, accum_out=sml[:, 0:1], r=[X1], w=["junk5", "sml"])
                kb.act(sml[:, 1:2], sml[:, 0:1], AF.Sqrt, bias=epsc, scale=1.0 / D, r=["sml"], w=["sml"])
                kb.op("dve", lambda e: e.reciprocal(out=sml[:, 2:3], in_=sml[:, 1:2]), r=["sml"], w=["sml"])
                kb.stt("dve", tmpf[:], x1_[:], sml[:, 2:3], B2[:], ALU.mult, ALU.mult, r=[X1, "sml", "B2"], w=["tmpf"])
                kb.tt("pool", h2res[:, tt_, :], tmpf[:], mrow[:, 1, :], ALU.add, r=["tmpf", "mrow"], w=[("h2", tt_)])
                for kc in range(8):
                    kb.mm(ps[2 + kc // 4][:, (kc % 4) * 128:(kc % 4 + 1) * 128], h2res[:, tt_, kc * 128:(kc + 1) * 128], ident_bf[:],
                          True, True, r=[("h2", tt_)], w=[PS(2 + kc // 4)])
                kb.act(h2T[:, 0:4, :], ps[2][:].rearrange("p (j v) -> p j v", v=128), AF.Copy, r=[PS(2)], w=["h2T"])
                kb.cp("dve", h2T[:, 4:8, :], ps[3][:].rearrange("p (j v) -> p j v", v=128), r=[PS(3)], w=["h2T"])
                for kc in range(8):
                    kb.mm(ps[4][:, 0:16], h2T[:, kc, :], wr[:, kc, :], kc == 0, kc == 7, r=["h2T", "wr"], w=[PS(4)])
                kb.op("dve", lambda e: e.tensor_reduce(out=sml[:, 3:4], in_=ps[4][:, 0:16], axis=AX.X, op=ALU.max),
                      r=[PS(4)], w=["sml"])
                kb.ts("dve", sml[:, 4:5], sml[:, 3:4], -1.0, None, ALU.mult, r=["sml"], w=["sml"])
                kb.memset("dve", sml[:, 5:6], 0.0, w=["sml"])
                kb.act(ex[:], ps[4][:, 0:16], AF.Exp, bias=sml[:, 4:5], scale=1.0, accum_out=sml[:, 5:6], r=[PS(4), "sml"], w=["ex", "sml"])
                kb.op("dve", lambda e: e.reciprocal(out=sml[:, 6:7], in_=sml[:, 5:6]), r=["sml"], w=["sml"])
                kb.ts("dve", aff_own[:, tt_, :], ex[:], sml[:, 6:7], None, ALU.mult, r=["ex", "sml"], w=[("aff", tt_)])
                kb.mm(ps[5][0:16, 0:128], aff_own[:, tt_, :], ident_f[:], True, True, r=[("aff", tt_)], w=[PS(5)])
                kb.cp("dve", affT[:, tt_ * 128:(tt_ + 1) * 128], ps[5][0:16, 0:128], r=[PS(5)], w=["affT"])
        kb.dma("sp", afft_d.ap(), affT[:], r=["affT"], w=["afft_d"])
        kb.flush()
        if upto <= 5:
            return nc
        NR = ncores * 16
        kb.coll(lambda e: e.collective_compute("AllGather", ALU.bypass, replica_groups=[list(range(ncores))],
                                               ins=[afft_d.ap().opt()], outs=[affall_d.ap().opt()]),
                r=["afft_d"], w=["affall_d"])
        affall = sb("affall", [128, NO])
        kb.memset("dve", affall[:], 0.0, w=["affall"])
        kb.dma("sp", affall[0:NR, :], affall_d.ap(), r=["affall_d"], w=["affall"])
        bj = sb("bj", [128, NO], BF16)
        bs = sb("bs", [128, 16])
        kb.memset("dve", bs[:, 0:1], 0.0, w=["bs"])
        kb.memset("dve", bs[:, 1:2], 2.0, w=["bs"])
        kb.memset("dve", bs[:, 2:3], 1.0, w=["bs"])
        for it in range(36):
            kb.memset("dve", bs[:, 3:4], 0.0, w=["bs"])
            kb.ts("dve", bj[:], affall[:], bs[:, 2:3], 0.0, ALU.is_ge, ALU.add, accum_out=bs[:, 3:4], r=["affall", "bs"], w=["bj", "bs"])
            kb.mm(ps[0][:, 0:1], pairg_f[:], bs[:, 3:4], True, True, r=["bs"], w=[PS(0)])
            kb.ts("dve", bs[:, 4:5], ps[0][:, 0:1], 1023.5, None, ALU.is_ge, r=[PS(0)], w=["bs"])
            kb.tt("dve", bs[:, 5:6], bs[:, 2:3], bs[:, 0:1], ALU.subtract, r=["bs"], w=["bs"])
            kb.tt("dve", bs[:, 6:7], bs[:, 1:2], bs[:, 2:3], ALU.subtract, r=["bs"], w=["bs"])
            kb.stt("dve", bs[:, 0:1], bs[:, 5:6], bs[:, 4:5], bs[:, 0:1], ALU.mult, ALU.add, r=["bs"], w=["bs"])
            kb.stt("dve", bs[:, 1:2], bs[:, 6:7], bs[:, 4:5], bs[:, 2:3], ALU.mult, ALU.add, r=["bs"], w=["bs"])
            kb.tt("dve", bs[:, 2:3], bs[:, 0:1], bs[:, 1:2], ALU.add, r=["bs"], w=["bs"])
            kb.ts("dve", bs[:, 2:3], bs[:, 2:3], 0.5, None, ALU.mult, r=["bs"], w=["bs"])
        thrB = sb("thrB", [128, 128])
        kb.cp("dve", thrB[:], bs[:, 0:1].to_broadcast([128, 128]), r=["bs"], w=["thrB"])
        kb.mm(ps[1][:, 0:16], thrB[:], sel_f[:], True, True, r=["thrB"], w=[PS(1)])
        thr = sb("thr", [128, 16])
        kb.cp("dve", thr[:], ps[1][:, 0:16], r=[PS(1)], w=["thr"])
        maskf = sb("maskf", [128, 32, 16])
        AFK = [("aff", t) for t in range(32)]
        kb.tt("dve", maskf[:], aff_own[:], thr[:].unsqueeze(1).to_broadcast([128, 32, 16]), ALU.is_ge, r=AFK + ["thr"], w=["maskf"])
        maskb = sb("maskb", [128, 512], BF16)
        kb.cp("dve", maskb[:], maskf[:].rearrange("p t e -> p (t e)"), r=["maskf"], w=["maskb"])
        kb.tt("dve", wgt[:], aff_own[:], maskf[:], ALU.mult, r=AFK + ["maskf"], w=["wgt"])
        kb.mm(ps[2][:], upper_bf[:], maskb[:], True, True, r=["maskb"], w=[PS(2)])
        kb.mm(ps[3][:], ones_bf[:], maskb[:], True, True, r=["maskb"], w=[PS(3)])
        tots = sb("tots", [128, 32, 16])
        base = sb("base", [128, 32, 16])
        kb.cp("dve", tots[:], ps[3][:].rearrange("p (t e) -> p t e", e=16), r=[PS(3)], w=["tots"])
        kb.memset("dve", base[:, 0, :], 0.0, w=["base"])
        for t in range(1, 32):
            kb.tt("dve", base[:, t, :], base[:, t - 1, :], tots[:, t - 1, :], ALU.add, r=["base", "tots"], w=["base"])
        slotf = sb("slotf", [128, 32, 16])
        kb.tt("dve", slotf[:], ps[2][:].rearrange("p (t e) -> p t e", e=16), base[:], ALU.add, r=[PS(2), "base"], w=["slotf"])
        kb.ts("dve", maskf[:], maskf[:], -100000.0, 100000.0, ALU.mult, ALU.add, r=["maskf"], w=["maskf"])
        kb.tt("dve", slotf[:], slotf[:], maskf[:], ALU.add, r=["slotf", "maskf"], w=["slotf"])
        kb.cp("dve", idx[:], slotf[:], r=["slotf"], w=["idx"])
        if debug:
            kb.dma("sp", dbg_d.ap()[:, 0:512], slotf[:].rearrange("p t e -> p (t e)"), r=["slotf"])
            kb.dma("sp", dbg_d.ap()[:, 512:1024], wgt[:].rearrange("p t e -> p (t e)"), r=["wgt"])
            kb.dma("sp", dbg_d.ap()[:, 1024:1040], thr[:], r=["thr"])
            kb.dma("sp", dbg_d.ap()[:, 1040:1056], bs[:], r=["bs"])
        for tt_ in range(32):
            for e_ in range(16):
                kb.dma_fn("pool", lambda eng, tt_=tt_, e_=e_: eng.indirect_dma_start(
                    out=xe_l[e_].ap(), out_offset=bass.IndirectOffsetOnAxis(ap=idx[:, tt_, e_:e_ + 1], axis=0),
                    in_=h2res[:, tt_, :], in_offset=None, bounds_check=kb.bcreg(eng), oob_is_err=False),
                    r=["idx", ("h2", tt_)], w=[])
        kb.flush()
    if upto <= 6:
        return nc

    with ExitStack() as st:
        sb = lambda name, shape, dt=F32: st.enter_context(nc.sbuf_tensor(name, list(shape), dt))
        ps = [st.enter_context(nc.psum_tensor(f"ps7_{i}", [128, 512], F32)) for i in range(8)]
        wg_ = sb("wg_", [128, 8, 2048], BF16)
        wu_ = sb("wu_", [128, 8, 2048], BF16)
        wd_ = sb("wd_", [128, 16, D], BF16)
        xtok = sb("xtok", [128, 8, D], BF16)
        xeT = sb("xeT", [128, 8, 1024], BF16)
        hidT = sb("hidT", [128, 16, 1024], BF16)
        sg = [sb(f"sg{i}", [128, 512]) for i in range(2)]
        ytok = [sb(f"ytok{i}", [128, D]) for i in range(2)]
        for e_ in range(16):
            wgv = w_gate.ap()[e_].rearrange("(j p) f -> p j f", p=128)
            wuv = w_up.ap()[e_].rearrange("(j p) f -> p j f", p=128)
            wdv = w_down.ap()[e_].rearrange("(j p) f -> p j f", p=128)
            kb.dma("sp", xtok[:], xe_l[e_].ap().rearrange("(a p) d -> p a d", p=128), w=["xtok"])
            for q4 in range(4):
                kb.dma("pool", wg_[:, q4 * 2:(q4 + 1) * 2, :], wgv[:, q4 * 2:(q4 + 1) * 2, :], w=[("wg_", q4)])
            for q4 in range(4):
                kb.dma("pool", wu_[:, q4 * 2:(q4 + 1) * 2, :], wuv[:, q4 * 2:(q4 + 1) * 2, :], w=[("wu_", q4)])
            for q4 in range(4):
                kb.dma("pool", wd_[:, q4 * 4:(q4 + 1) * 4, :], wdv[:, q4 * 4:(q4 + 1) * 4, :], w=[("wd_", q4)])
            for kc in range(8):
                b0 = (kc % 2) * 2
                for a in range(8):
                    kb.mm(ps[b0 + a // 4][:, (a % 4) * 128:(a % 4 + 1) * 128], xtok[:, a, kc * 128:(kc + 1) * 128], ident_bf[:],
                          True, True, r=["xtok"], w=[PS(b0 + a // 4)])
                kb.act(xeT[:, kc, 0:512], ps[b0][:], AF.Copy, r=[PS(b0)], w=["xeT"])
                kb.cp("dve", xeT[:, kc, 512:1024], ps[b0 + 1][:], r=[PS(b0 + 1)], w=["xeT"])
            WG = [("wg_", q) for q in range(4)]
            WU = [("wu_", q) for q in range(4)]
            WD = [("wd_", q) for q in range(4)]
            it = 0
            for fc in range(16):
                for sh in range(2):
                    pg, pu = 4 + it % 2, 6 + it % 2
                    s_ = sg[it % 2]
                    it += 1
                    for kc in range(8):
                        kb.mm(ps[pg][:], wg_[:, kc, fc * 128:(fc + 1) * 128], xeT[:, kc, sh * 512:(sh + 1) * 512], kc == 0, kc == 7,
                              r=WG + ["xeT"], w=[PS(pg)])
                    for kc in range(8):
                        kb.mm(ps[pu][:], wu_[:, kc, fc * 128:(fc + 1) * 128], xeT[:, kc, sh * 512:(sh + 1) * 512], kc == 0, kc == 7,
                              r=WU + ["xeT"], w=[PS(pu)])
                    kb.act(s_[:], ps[pg][:], AF.Silu, r=[PS(pg)], w=[("sg", it % 2)])
                    kb.tt("dve", hidT[:, fc, sh * 512:(sh + 1) * 512], s_[:], ps[pu][:], ALU.mult, r=[("sg", it % 2), PS(pu)], w=["hidT"])
            for a in range(8):
                y_ = ytok[a % 2]
                for hf in range(2):
                    pb = (a % 2) * 2 + hf
                    for fc in range(16):
                        kb.mm(ps[pb][:], hidT[:, fc, a * 128:(a + 1) * 128], wd_[:, fc, hf * 512:(hf + 1) * 512], fc == 0, fc == 15,
                              r=WD + ["hidT"], w=[PS(pb)])
                    if hf == 0:
                        kb.act(y_[:, 0:512], ps[pb][:], AF.Copy, r=[PS(pb)], w=[("ytok", a % 2)])
                    else:
                        kb.cp("dve", y_[:, 512:1024], ps[pb][:], r=[PS(pb)], w=[("ytok", a % 2)])
                kb.dma("sp", ye_l[e_].ap()[a * 128:(a + 1) * 128, :], y_[:], r=[("ytok", a % 2)], w=[("ye", e_)])
        kb.flush()
    if upto <= 7:
        return nc

    with ExitStack() as st:
        sb = lambda name, shape, dt=F32: st.enter_context(nc.sbuf_tensor(name, list(shape), dt))
        gt2b = sb("gt2b", [128, D])
        kb.dma("sp", gt2b[:], modr_d.ap()[:, 3 * D:4 * D], w=["gt2b"])
        gfinb = sb("gfinb", [128, D])
        kb.dma("sp", gfinb[:], bass.AP(g_final, 0, [[0, 128], [1, D]]), w=["gfinb"])
        Gb = [sb(f"Gb{i}", [128, D]) for i in range(4)]
        for i in range(4):
            kb.memset("pool", Gb[i][:], 0.0, w=[("Gb", i)])
        acc = sb("acc", [128, D])
        x1b = [sb(f"x1b{i}", [128, D]) for i in range(2)]
        outb = [sb(f"outb{i}", [128, D]) for i in range(2)]
        junk = sb("junk8", [128, D], BF16)
        sml = sb("sml8", [128, 4])
        gi = 0
        for tt_ in range(32):
            x1_ = x1b[tt_ % 2]
            o_ = outb[tt_ % 2]
            X1, OB = ("x1b", tt_ % 2), ("outb", tt_ % 2)
            kb.dma("sp", x1_[:], x1_d.ap()[tt_ * 128:(tt_ + 1) * 128, :], w=[X1])
            for e_ in range(16):
                k = gi % 4
                gi += 1
                kb.dma_fn("pool", lambda eng, tt_=tt_, e_=e_, k=k: eng.indirect_dma_start(
                    out=Gb[k][:], out_offset=None, in_=ye_l[e_].ap(),
                    in_offset=bass.IndirectOffsetOnAxis(ap=idx[:, tt_, e_:e_ + 1], axis=0),
                    bounds_check=kb.bcreg(eng), oob_is_err=False), r=[], w=[("Gb", k)])
                if e_ == 0:
                    kb.ts("dve", acc[:], Gb[k][:], wgt[:, tt_, e_:e_ + 1], None, ALU.mult, r=[("Gb", k)], w=["acc"])
                else:
                    kb.stt("dve", acc[:], Gb[k][:], wgt[:, tt_, e_:e_ + 1], acc[:], ALU.mult, ALU.add, r=[("Gb", k), "acc"], w=["acc"])
            kb.tt("dve", acc[:], acc[:], gt2b[:], ALU.mult, r=["acc", "gt2b"], w=["acc"])
            kb.tt("dve", x1_[:], x1_[:], acc[:], ALU.add, r=[X1, "acc"], w=[X1])
            kb.memset("dve", sml[:, 0:1], 0.0, w=["sml8"])
            kb.act(junk[:], x1_[:], AF.Square, accum_out=sml[:, 0:1], r=[X1], w=["junk8", "sml8"])
            kb.act(sml[:, 1:2], sml[:, 0:1], AF.Sqrt, bias=epsc, scale=1.0 / D, r=["sml8"], w=["sml8"])
            kb.op("dve", lambda e: e.reciprocal(out=sml[:, 2:3], in_=sml[:, 1:2]), r=["sml8"], w=["sml8"])
            kb.stt("dve", o_[:], x1_[:], sml[:, 2:3], gfinb[:], ALU.mult, ALU.mult, r=[X1, "sml8", "gfinb"], w=[OB])
            kb.dma("sp", out_d.ap()[tt_ * 128:(tt_ + 1) * 128, :], o_[:], r=[OB])
        kb.flush()
    return nc


def make_in_maps(inputs, cores=range(NCORES)):
    f = lambda a: np.ascontiguousarray(a, dtype=np.float32)
    shared = {
        "w_ada": f(inputs["w_ada"][0]), "b_ada": f(inputs["b_ada"][0]).reshape(1, -1),
        "g_mix": f(inputs["g_norm_mix"][0]).reshape(1, -1), "w_in": f(inputs["w_in"][0]),
        "g_q": f(inputs["g_q_lat"][0]).reshape(1, -1), "g_kv": f(inputs["g_kv_lat"][0]).reshape(1, -1),
        "w_uq": f(inputs["w_uq"][0]), "w_ukv": f(inputs["w_ukv"][0]), "rel_bias": f(inputs["rel_bias"]),
        "w_out": f(inputs["w_out"][0]), "g_ffn": f(inputs["g_norm_ffn"][0]).reshape(1, -1),
        "w_router": f(inputs["w_router"][0]), "w_gate": f(inputs["w_gate"][0]), "w_up": f(inputs["w_up"][0]),
        "w_down": f(inputs["w_down"][0]), "g_final": f(inputs["g_final"]).reshape(1, -1),
    }
    maps = []
    for core in cores:
        b, half = core // 2, core % 2
        xb = np.asarray(inputs["x"][b], dtype=np.float32)
        pb = np.asarray(inputs["positions"][b], dtype=np.int32)
        if half:
            xb = xb[::-1]
            pb = pb[::-1]
        m = dict(shared)
        m["xT"] = np.ascontiguousarray(xb.T)
        m["xo"] = np.ascontiguousarray(xb[:NO])
        m["pos"] = np.ascontiguousarray(pb).reshape(1, S)
        m["ccol"] = np.ascontiguousarray(np.asarray(inputs["c"][b], dtype=np.float32).reshape(8, 128).T)
        for k, v in _consts(core).items():
            m["c_" + k] = v
        maps.append(m)
    return maps


def kernel(**inputs):
    nc = build()
    maps = make_in_maps(inputs)
    res = run_bass_kernel_spmd(nc, maps, core_ids=list(range(NCORES)))
    out = np.empty((4, S, D), np.float32)
    for core in range(NCORES):
        b, half = core // 2, core % 2
        o = np.asarray(res.results[core]["out"], dtype=np.float32)
        if half:
            out[b, NO:] = o[::-1]
        else:
            out[b, :NO] = o
    return out
```
